# Optimizing a Trainium2 kernel written in Bass

```python
import math
import jax, jax.numpy as jnp
from jax import lax
import numpy as np

D_MODEL = 1024
BATCH = 32
SEQ = 2048
DEPTH = 1

HEAD_DIM = 64
N_Q_HEADS = D_MODEL // HEAD_DIM
N_KV_HEADS = N_Q_HEADS // 8
Q_PER_KV = N_Q_HEADS // N_KV_HEADS
WINDOW = 128
ATTN_BLOCK = 128
NUM_BUCKETS = 32
MAX_DISTANCE = 128
D_INNER = 2 * D_MODEL
SSM_HEAD_DIM = 64
N_SSM_HEADS = D_INNER // SSM_HEAD_DIM
N_SSM_GROUPS = 4
HEADS_PER_GROUP = N_SSM_HEADS // N_SSM_GROUPS
D_STATE = 128
CONV_WIDTH = 4
CHUNK = 128
CONV_CH = D_INNER + 2 * N_SSM_GROUPS * D_STATE
N_EXPERT_GROUPS = 4
EXPERTS_PER_GROUP = 8
N_EXPERTS = N_EXPERT_GROUPS * EXPERTS_PER_GROUP
TOP_K = 2
D_EXPERT = D_MODEL // 2
MOE_BLOCK = 128
DEEPNORM_ALPHA = (2.0 * DEPTH) ** 0.25
DEEPNORM_BETA = (8.0 * DEPTH) ** -0.25
LN_EPS = 1e-5
PROJ_SIZES = (N_Q_HEADS * HEAD_DIM, N_KV_HEADS * HEAD_DIM, N_KV_HEADS * HEAD_DIM,
              D_INNER, D_INNER, N_SSM_GROUPS * D_STATE, N_SSM_GROUPS * D_STATE,
              N_SSM_HEADS, D_MODEL, D_MODEL)
PROJ_COLS = sum(PROJ_SIZES)
PROJ_SPLITS = tuple(int(v) for v in np.cumsum(PROJ_SIZES)[:-1])

kernel_name = "hybrid_swa_sink_ssd_hmoe_deepnorm"


def layer_norm(x, g, b):
    xf = x.astype(jnp.float32)
    mu = jnp.mean(xf, -1, keepdims=True)
    var = jnp.mean(jnp.square(xf - mu), -1, keepdims=True)
    y = (xf - mu) * lax.rsqrt(var + LN_EPS) * g.astype(jnp.float32) + b.astype(jnp.float32)
    return y.astype(x.dtype)


def t5_causal_bucket(dist):
    max_exact = NUM_BUCKETS // 2
    d_f = jnp.maximum(dist, 1).astype(jnp.float32)
    large = max_exact + (jnp.log(d_f / max_exact) / math.log(MAX_DISTANCE / max_exact)
                         * (NUM_BUCKETS - max_exact)).astype(jnp.int32)
    large = jnp.minimum(large, NUM_BUCKETS - 1)
    return jnp.where(dist < max_exact, dist, large)


def banded_relative_bias(table):
    qi = jnp.arange(ATTN_BLOCK)[:, None]
    kj = jnp.arange(2 * ATTN_BLOCK)[None, :]
    dist = qi + ATTN_BLOCK - kj
    bias = table[t5_causal_bucket(jnp.clip(dist, 0, None))].astype(jnp.float32)
    return jnp.transpose(bias, (2, 0, 1)).reshape(N_KV_HEADS, Q_PER_KV, ATTN_BLOCK, 2 * ATTN_BLOCK)


def sliding_window_attention(q, k, v, sink, pos_bias):
    b, s = q.shape[:2]
    nb = s // ATTN_BLOCK
    scale = HEAD_DIM ** -0.5
    qb = q.reshape(b, nb, ATTN_BLOCK, N_KV_HEADS, Q_PER_KV, HEAD_DIM)

    def band(t):
        tb = t.reshape(b, nb, ATTN_BLOCK, N_KV_HEADS, HEAD_DIM)
        prev = jnp.concatenate([jnp.zeros_like(tb[:, :1]), tb[:, :-1]], axis=1)
        return jnp.concatenate([prev, tb], axis=2)

    kw, vw = band(k), band(v)
    qi = jnp.arange(ATTN_BLOCK)[:, None]
    kj = jnp.arange(2 * ATTN_BLOCK)[None, :]
    dist = qi + ATTN_BLOCK - kj
    in_window = (dist >= 0) & (dist < WINDOW)
    sink_b = sink.astype(jnp.float32).reshape(N_KV_HEADS, Q_PER_KV)[None, :, :, None, None]

    def one_block(args):
        qblk, kblk, vblk, i = args
        sc = jnp.einsum('bqkgd,bskd->bkgqs', qblk, kblk).astype(jnp.float32) * scale + pos_bias
        key_ok = in_window & ((kj >= ATTN_BLOCK) | (i > 0))
        sc = jnp.where(key_ok, sc, -jnp.inf)
        m = jnp.maximum(jnp.max(sc, -1, keepdims=True), sink_b)
        p = jnp.exp(sc - m)
        denom = jnp.sum(p, -1, keepdims=True) + jnp.exp(sink_b - m)
        return jnp.einsum('bkgqs,bskd->bqkgd', (p / denom).astype(vblk.dtype), vblk)

    out = lax.map(one_block, (jnp.moveaxis(qb, 1, 0), jnp.moveaxis(kw, 1, 0),
                              jnp.moveaxis(vw, 1, 0), jnp.arange(nb)))
    return jnp.moveaxis(out, 0, 1).reshape(b, s, N_Q_HEADS * HEAD_DIM)


def causal_depthwise_conv(x, w, bias):
    y = lax.conv_general_dilated(x, w[:, None, :].astype(x.dtype), window_strides=(1,),
                                 padding=[(CONV_WIDTH - 1, 0)],
                                 dimension_numbers=('NWC', 'WIO', 'NWC'),
                                 feature_group_count=x.shape[-1])
    return y + bias.astype(x.dtype)


def ssd_chunked_scan(xs, dt, a, bm, cm):
    b, s = xs.shape[:2]
    nc = s // CHUNK

    def chunks(t):
        return jnp.moveaxis(t.reshape((b, nc, CHUNK) + t.shape[2:]), 1, 0)

    causal = jnp.tril(jnp.ones((CHUNK, CHUNK), bool))[None, :, :, None, None]

    def step(state, inp):
        xc, dtc, bc, cc = inp
        acum = jnp.cumsum(dtc * a, axis=1)
        seg = acum[:, :, None] - acum[:, None, :]
        decay = jnp.exp(jnp.where(causal, seg, -jnp.inf))
        xdt = xc * dtc[..., None]
        cb = jnp.einsum('btgn,bsgn->btsg', cc, bc)
        y_diag = jnp.einsum('btsg,btsge,bsgep->btgep', cb, decay, xdt)
        y_off = jnp.einsum('btgn,bgepn->btgep', cc, state) * jnp.exp(acum)[..., None]
        to_end = jnp.exp(acum[:, -1:] - acum)
        new_state = (state * jnp.exp(acum[:, -1])[..., None, None]
                     + jnp.einsum('bsgn,bsge,bsgep->bgepn', bc, to_end, xdt))
        return new_state, y_diag + y_off

    state0 = jnp.zeros((b, N_SSM_GROUPS, HEADS_PER_GROUP, SSM_HEAD_DIM, D_STATE), jnp.float32)
    _, y = lax.scan(step, state0, (chunks(xs), chunks(dt), chunks(bm), chunks(cm)))
    return jnp.moveaxis(y, 0, 1).reshape(xs.shape)


def gated_group_rmsnorm(y, z, g):
    b, s = y.shape[:2]
    h = y.astype(jnp.float32) * jax.nn.silu(z.astype(jnp.float32))
    h = h.reshape(b, s, N_SSM_GROUPS, D_INNER // N_SSM_GROUPS)
    h = h * lax.rsqrt(jnp.mean(jnp.square(h), -1, keepdims=True) + LN_EPS)
    return h.reshape(b, s, D_INNER) * g.astype(jnp.float32)


def hybrid_mixer(u, pos_bias, w_in, b_gate, sink, conv_w, conv_b, dt_bias, a_log, d_skip,
                 ssm_norm_g, w_attn_out, w_ssm_out, w_out):
    b, s = u.shape[:2]
    proj = u @ w_in
    q, k, v, z, xs, bm, cm, dt, ga, gs = jnp.split(proj, PROJ_SPLITS, axis=-1)
    attn = sliding_window_attention(q.reshape(b, s, N_KV_HEADS, Q_PER_KV, HEAD_DIM),
                                    k.reshape(b, s, N_KV_HEADS, HEAD_DIM),
                                    v.reshape(b, s, N_KV_HEADS, HEAD_DIM), sink, pos_bias)
    attn_branch = attn @ w_attn_out
    xbc = jax.nn.silu(causal_depthwise_conv(jnp.concatenate([xs, bm, cm], -1), conv_w, conv_b))
    xs, bm, cm = jnp.split(xbc, [D_INNER, D_INNER + N_SSM_GROUPS * D_STATE], axis=-1)
    xs_h = xs.astype(jnp.float32).reshape(b, s, N_SSM_GROUPS, HEADS_PER_GROUP, SSM_HEAD_DIM)
    bm = bm.astype(jnp.float32).reshape(b, s, N_SSM_GROUPS, D_STATE)
    cm = cm.astype(jnp.float32).reshape(b, s, N_SSM_GROUPS, D_STATE)
    dtv = jax.nn.softplus(dt.astype(jnp.float32) + dt_bias.astype(jnp.float32))
    dtv = dtv.reshape(b, s, N_SSM_GROUPS, HEADS_PER_GROUP)
    a = -jnp.exp(a_log.astype(jnp.float32)).reshape(N_SSM_GROUPS, HEADS_PER_GROUP)
    y = ssd_chunked_scan(xs_h, dtv, a, bm, cm)
    y = y + d_skip.astype(jnp.float32).reshape(N_SSM_GROUPS, HEADS_PER_GROUP)[..., None] * xs_h
    y = gated_group_rmsnorm(y.reshape(b, s, D_INNER), z, ssm_norm_g).astype(u.dtype)
    ssm_branch = y @ w_ssm_out
    gate = jax.nn.sigmoid(jnp.concatenate([ga, gs], -1) + b_gate)
    gate_a, gate_s = jnp.split(gate, [D_MODEL], axis=-1)
    return (gate_a * attn_branch + gate_s * ssm_branch) @ w_out


def hierarchical_moe(h, w_group_router, w_expert_router, w_gate_e, w_up_e, w_down_e):
    b, s, d = h.shape
    t = b * s
    xf = h.reshape(t, d)
    g_prob = jax.nn.softmax((xf @ w_group_router).astype(jnp.float32), -1)
    grp = jnp.argmax(g_prob, -1)
    p_grp = jnp.take_along_axis(g_prob, grp[:, None], -1)[:, 0]
    e_logit = (xf @ w_expert_router).astype(jnp.float32).reshape(t, N_EXPERT_GROUPS, EXPERTS_PER_GROUP)
    e_logit = jnp.take_along_axis(e_logit, grp[:, None, None], axis=1)[:, 0]
    top_p, top_i = lax.top_k(jax.nn.softmax(e_logit, -1), TOP_K)
    top_p = top_p / jnp.sum(top_p, -1, keepdims=True)
    gate_w = (p_grp[:, None] * top_p).reshape(-1)
    eid = (grp[:, None] * EXPERTS_PER_GROUP + top_i).reshape(-1).astype(jnp.int32)
    tok = jnp.repeat(jnp.arange(t, dtype=jnp.int32), TOP_K)
    n_assign = t * TOP_K
    order = jnp.argsort(eid)
    se = eid[order]
    counts = jnp.bincount(eid, length=N_EXPERTS).astype(jnp.int32)
    starts = jnp.cumsum(counts) - counts
    padded = (counts + MOE_BLOCK - 1) // MOE_BLOCK * MOE_BLOCK
    pends = jnp.cumsum(padded)
    pstarts = pends - padded
    dest = pstarts[se] + (jnp.arange(n_assign, dtype=jnp.int32) - starts[se])
    n_rows = (n_assign // MOE_BLOCK + N_EXPERTS) * MOE_BLOCK
    row_tok = jnp.full((n_rows,), t, jnp.int32).at[dest].set(tok[order])
    row_w = jnp.zeros((n_rows,), jnp.float32).at[dest].set(gate_w[order])
    n_blk = n_rows // MOE_BLOCK
    block_e = jnp.minimum(jnp.searchsorted(pends, jnp.arange(n_blk, dtype=jnp.int32) * MOE_BLOCK,
                                           side='right'), N_EXPERTS - 1)
    xpad = jnp.concatenate([xf, jnp.zeros((1, d), xf.dtype)], 0)
    xr = xpad[row_tok].reshape(n_blk, MOE_BLOCK, d)

    def expert_block(args):
        xb, e = args
        hid = jax.nn.silu(xb @ w_gate_e[e]) * (xb @ w_up_e[e])
        return hid @ w_down_e[e]

    yr = lax.map(expert_block, (xr, block_e)).reshape(n_rows, d)
    y = jnp.zeros((t + 1, d), yr.dtype).at[row_tok].add(yr * row_w[:, None].astype(yr.dtype))[:t]
    return y.reshape(b, s, d)


def setup_inputs(seed: int = 0) -> dict:
    key = jax.random.key(seed)
    ks = jax.random.split(key, 26)
    f32 = jnp.float32

    def nrm(k, shape, scale):
        return jax.random.normal(k, shape, f32) * scale

    beta = DEEPNORM_BETA
    col_scale = jnp.concatenate([
        jnp.full((sz,), beta if i in (2, 4) else 1.0, f32) for i, sz in enumerate(PROJ_SIZES)])
    dt0 = jnp.exp(jax.random.uniform(ks[9], (DEPTH, N_SSM_HEADS), f32, math.log(1e-3), math.log(1e-1)))
    return {
        "x": nrm(ks[0], (BATCH, SEQ, D_MODEL), 1.0),
        "ln_in_g": 1.0 + nrm(ks[1], (D_MODEL,), 0.02),
        "ln_in_b": nrm(ks[2], (D_MODEL,), 0.02),
        "rel_bias": nrm(ks[3], (NUM_BUCKETS, N_Q_HEADS), 0.2),
        "w_in": nrm(ks[4], (DEPTH, D_MODEL, PROJ_COLS), D_MODEL ** -0.5) * col_scale,
        "b_gate": nrm(ks[5], (DEPTH, 2 * D_MODEL), 0.02),
        "attn_sink": nrm(ks[6], (DEPTH, N_Q_HEADS), 0.5),
        "conv_w": nrm(ks[7], (DEPTH, CONV_WIDTH, CONV_CH), CONV_WIDTH ** -0.5),
        "conv_b": nrm(ks[8], (DEPTH, CONV_CH), 0.02),
        "dt_bias": dt0 + jnp.log(-jnp.expm1(-dt0)),
        "a_log": jnp.log(jax.random.uniform(ks[10], (DEPTH, N_SSM_HEADS), f32, 1.0, 16.0)),
        "d_skip": 1.0 + nrm(ks[11], (DEPTH, N_SSM_HEADS), 0.1),
        "ssm_norm_g": 1.0 + nrm(ks[12], (DEPTH, D_INNER), 0.02),
        "w_attn_out": nrm(ks[13], (DEPTH, N_Q_HEADS * HEAD_DIM, D_MODEL), (N_Q_HEADS * HEAD_DIM) ** -0.5 * beta),
        "w_ssm_out": nrm(ks[14], (DEPTH, D_INNER, D_MODEL), D_INNER ** -0.5 * beta),
        "w_out": nrm(ks[15], (DEPTH, D_MODEL, D_MODEL), D_MODEL ** -0.5 * beta),
        "ln1_g": 1.0 + nrm(ks[16], (DEPTH, D_MODEL), 0.02),
        "ln1_b": nrm(ks[17], (DEPTH, D_MODEL), 0.02),
        "w_group_router": nrm(ks[18], (DEPTH, D_MODEL, N_EXPERT_GROUPS), D_MODEL ** -0.5),
        "w_expert_router": nrm(ks[19], (DEPTH, D_MODEL, N_EXPERTS), D_MODEL ** -0.5),
        "w_gate_e": nrm(ks[20], (DEPTH, N_EXPERTS, D_MODEL, D_EXPERT), D_MODEL ** -0.5 * beta),
        "w_up_e": nrm(ks[21], (DEPTH, N_EXPERTS, D_MODEL, D_EXPERT), D_MODEL ** -0.5 * beta),
        "w_down_e": nrm(ks[22], (DEPTH, N_EXPERTS, D_EXPERT, D_MODEL), D_EXPERT ** -0.5 * beta),
        "ln2_g": 1.0 + nrm(ks[23], (DEPTH, D_MODEL), 0.02),
        "ln2_b": nrm(ks[24], (DEPTH, D_MODEL), 0.02),
    }


def reference(x, ln_in_g, ln_in_b, rel_bias, w_in, b_gate, attn_sink, conv_w, conv_b, dt_bias,
              a_log, d_skip, ssm_norm_g, w_attn_out, w_ssm_out, w_out, ln1_g, ln1_b,
              w_group_router, w_expert_router, w_gate_e, w_up_e, w_down_e, ln2_g, ln2_b):
    h = layer_norm(x, ln_in_g, ln_in_b)
    pos_bias = banded_relative_bias(rel_bias)
    for l in range(DEPTH):
        mix = hybrid_mixer(h, pos_bias, w_in[l], b_gate[l], attn_sink[l], conv_w[l], conv_b[l],
                           dt_bias[l], a_log[l], d_skip[l], ssm_norm_g[l], w_attn_out[l],
                           w_ssm_out[l], w_out[l])
        h = layer_norm(DEEPNORM_ALPHA * h + mix, ln1_g[l], ln1_b[l])
        ffn = hierarchical_moe(h, w_group_router[l], w_expert_router[l], w_gate_e[l], w_up_e[l], w_down_e[l])
        h = layer_norm(DEEPNORM_ALPHA * h + ffn, ln2_g[l], ln2_b[l])
    return h
```

```python
import math
from contextlib import ExitStack
import numpy as np
import concourse.bass as bass
import concourse.mybir as mybir
from concourse.bass_utils import run_bass_kernel_spmd

F32 = mybir.dt.float32
BF16 = mybir.dt.bfloat16
I32 = mybir.dt.int32
AF = mybir.ActivationFunctionType
ALU = mybir.AluOpType
AX = mybir.AxisListType

P = 128
D = 1024
NCORE = 8
NSEQ = 4
SEQ = 2048
NT = NSEQ * SEQ // P
NST = NT // 4
CAP = 768
NSLOT = 32 * CAP
EPS = 1e-5
ALPHA = 2.0 ** 0.25
NMIX = 28
RING = 3

OFF_Q, OFF_K, OFF_V, OFF_Z, OFF_XS, OFF_B, OFF_C, OFF_DT, OFF_GA, OFF_GS = (
    0, 1024, 1152, 1280, 3328, 5376, 5888, 6400, 6432, 7456)


class Sched:
    def __init__(self, nc, es, nds=32):
        self.nc = nc
        self.ops = {e: [] for e in ("pe", "act", "dve", "pool", "sp")}
        self.sem = {e: es.enter_context(nc.semaphore("s_" + e)) for e in ("pe", "act", "dve")}
        self.cnt = {e: 0 for e in ("pe", "act", "dve")}
        self.known = {e: {} for e in self.ops}
        self.dsem = [es.enter_context(nc.semaphore("d%d" % i)) for i in range(nds)]
        self.dcnt = [0] * nds
        self.dnext = {"sp": 0, "pool": 0}
        self.dpool = {"sp": list(range(0, nds // 2)), "pool": list(range(nds // 2, nds))}
        self.bufs = {}

    def _semobj(self, sk):
        return self.sem[sk] if isinstance(sk, str) else self.dsem[sk[1]]

    def _wait(self, eng, sk, v, src):
        if src == eng and eng == "pe":
            return
        if self.known[eng].get(sk, 0) >= v:
            return
        self.known[eng][sk] = v
        so = self._semobj(sk)
        self.ops[eng].append(lambda e, so=so, v=v: e.wait_ge(so, v))

    def op(self, eng, fn, r=(), w=(), waw=True):
        for k in r:
            b = self.bufs.setdefault(k, {"w": {}, "r": {}})
            for sk, (v, src) in b["w"].items():
                self._wait(eng, sk, v, src)
            if isinstance(k, str) and (k == "T0" or (k[0] == "B" and k[1:].isdigit())):
                for sk, (v, src) in b["r"].items():
                    if src != eng:
                        self._wait(eng, sk, v, src)
        for k in w:
            b = self.bufs.setdefault(k, {"w": {}, "r": {}})
            if waw:
                for sk, (v, src) in b["w"].items():
                    self._wait(eng, sk, v, src)
            for sk, (v, src) in b["r"].items():
                self._wait(eng, sk, v, src)
        if eng in self.cnt:
            self.cnt[eng] += 1
            v = self.cnt[eng]
            sk = eng
            so = self.sem[eng]
            self.ops[eng].append(lambda e, fn=fn, so=so: fn(e).then_inc(so, 1))
            src = eng
        else:
            pl = self.dpool[eng]
            i = pl[self.dnext[eng]]
            self.dnext[eng] = (self.dnext[eng] + 1) % len(pl)
            if self.dcnt[i] > 0:
                self._wait(eng, ("d", i), self.dcnt[i], "dma")
            self.dcnt[i] += 16
            v = self.dcnt[i]
            sk = ("d", i)
            so = self.dsem[i]
            self.ops[eng].append(lambda e, fn=fn, so=so: fn(e).then_inc(so, 16))
            src = "dma"
        for k in r:
            b = self.bufs[k]
            if b["r"].get(sk, (0, None))[0] < v:
                b["r"][sk] = (v, src)
        for k in w:
            b = self.bufs[k]
            if waw:
                b["w"] = {sk: (v, src)}
                b["r"] = {}
            else:
                b["w"][sk] = (v, src)

    def pe(self, fn, r=(), w=()):
        self.op("pe", fn, r, w)

    def act(self, fn, r=(), w=()):
        self.op("act", fn, r, w)

    def dve(self, fn, r=(), w=()):
        self.op("dve", fn, r, w)

    def finish(self):
        for i, c in enumerate(self.dcnt):
            if c > 0:
                self._wait("sp", ("d", i), c, "dma")
        for e in ("pe", "act", "dve"):
            if self.cnt[e] > 0:
                self._wait("sp", e, self.cnt[e], e)


class _Stop(Exception):
    pass


def build_program(nst=NST, nexp=32, debug=False, stop=None):
    nc = bass.Bass("TRN2", target_bir_lowering=False)
    es = ExitStack()
    S = Sched(nc, es)

    def din(name, shape, dt=F32):
        return nc.dram_tensor(name, list(shape), dt, kind="ExternalInput").ap()

    x_d = din("x", [NT * P, D])
    cst_d = din("cst", [P, 640])
    lnbc_d = din("lnbc", [6, D])
    lncol_d = din("lncol", [P, 16])
    biasT_d = din("biasT", [P, 4096])
    maskT_d = din("maskT", [P, 256])
    small_d = din("small", [4, 32])
    convw_d = din("convw", [P, 96])
    convb_d = din("convb", [P, 24])
    bgate_d = din("bgate", [P, 16])
    normg_d = din("normg", [1, 2048])
    wr_d = din("wr", [P, 8 * 36])
    ebase_d = din("ebase", [P, 32])
    wall_d = din("wall", [NMIX, P, 4096])
    wexp_d = din("wexp", [96, P, 4096])
    out_d = nc.dram_tensor("out", [NT * P, D], F32, kind="ExternalOutput").ap()
    H1_d = nc.dram_tensor("H1s", [NT * P, D], F32, kind="Internal").ap()
    XG_d = nc.dram_tensor("XGs", [NSLOT, D], BF16, kind="Internal").ap()
    YG_d = nc.dram_tensor("YGs", [NSLOT, D], F32, kind="Internal").ap()

    if debug:
        dbg_b = nc.dram_tensor("dbg_b", [16, P, 4096], BF16, kind="ExternalOutput").ap()
        dbg_f = nc.dram_tensor("dbg_f", [16, P, 1024], F32, kind="ExternalOutput").ap()

    def dump_b(i, tile, key):
        if debug:
            dma_sp(dbg_b[i], tile[:], [key], [("dbgb", i)])

    def dump_f(i, tile, keys):
        if debug:
            dma_sp(dbg_f[i], tile[:], keys, [("dbgf", i)])

    def sb(name, shape, dt):
        return es.enter_context(nc.sbuf_tensor("sb_" + name, list(shape), dt))

    def psb(name, shape, dt):
        return es.enter_context(nc.psum_tensor("ps_" + name, list(shape), dt))

    cst = sb("cst", [P, 640], F32)
    cstb = sb("cstb", [P, 640], BF16)
    lbc = [sb("lbc%d" % i, [P, D], F32) for i in range(4)]
    lncol = sb("lncol", [P, 16], F32)
    bias8 = sb("bias8", [P, 4096], BF16)
    maskT = sb("maskT", [P, 256], F32)
    smallbc = sb("smallbc", [P, 4, 32], F32)
    esink = sb("esink", [P, 16], F32)
    abc = sb("abc", [P, 32], F32)
    convw = sb("convw", [P, 96], F32)
    convb = sb("convb", [P, 24], F32)
    bgate = sb("bgate", [P, 16], F32)
    wr = sb("wr", [P, 8 * 36], F32)
    ebase = sb("ebase", [P, 32], F32)
    U = [sb("U%d" % i, [P, 4096], BF16) for i in range(6)]
    Fp = [sb("F%d" % i, [P, D], F32) for i in range(6)]
    wbuf = [sb("wb%d" % i, [P, 4096], BF16) for i in range(RING)]
    v_aug = sb("vaug", [P, 5, 2, 128], BF16)
    kT = sb("kT", [P, 2, 640], BF16)
    PT = sb("PT", [P, 2, 2, 512], BF16)
    attn_tok = sb("attntok", [P, 512], BF16)
    dt_t = sb("dt", [P, 4, 32], F32)
    da_t = sb("da", [P, 4, 32], F32)
    hist = sb("hist", [P, 24, 3], F32)
    xsT_g = sb("xsTg", [P, 4, 512], BF16)
    BT_g = sb("BTg", [P, 512], BF16)
    CT_g = sb("CTg", [P, 512], BF16)
    xsB = sb("xsB", [P, 4, 640], BF16)
    sz = sb("sz", [P, 4, 512], BF16)
    dab = sb("dab", [P, 8], BF16)
    Whi = sb("Whi", [P, 8, 128], BF16)
    MT = sb("MT", [P, 8, 128], BF16)
    CBm = sb("CBm", [P, 1, 128], F32)
    Dm = sb("Dm", [P, 8, 128], BF16)
    ng = sb("ng", [P, 512], F32)
    wst = sb("wst", [P, 8], F32)
    xdt = sb("xdt", [P, 8, 64], BF16)
    xw = sb("xw", [P, 8, 64], BF16)
    yn = sb("yn", [P, 512], BF16)
    eac = sb("eac", [P, 16], F32)
    state_f = sb("statef", [P, 4, 512], F32)
    state_b = sb("stateb", [P, 4, 512], BF16)
    st6 = sb("st6", [P, 12], F32)
    mv = sb("mv", [P, 2], F32)
    rs = sb("rs", [P, 1], F32)
    rstd = sb("rstd", [P, 1], F32)
    nmr = sb("nmr", [P, 1], F32)
    ss = sb("ss", [P, 1], F32)
    den = sb("den", [P, 8], F32)
    rden = sb("rden", [P, 8], F32)
    h1b = sb("h1b", [P, D], BF16)
    h1T = sb("h1T", [P, 8, 128], F32)
    lg = sb("lg", [P, 36], F32)
    rt = sb("rt", [P, 160], F32)
    top8 = sb("top8", [P, 8], F32)
    mb = sb("mb", [P, 32], BF16)
    base = sb("base", [P, 32], F32)
    idx_all = sb("idxall", [P, NT, 2], I32)
    w_all = sb("wall_s", [P, NT, 2], F32)

    Bk = [psb("B%d" % i, [P, 512], F32) for i in range(7)]
    T0 = psb("T0", [P, 1024], BF16)

    def v3(t, b):
        return t[:].rearrange("p (a b) -> p a b", b=b)

    T0v = v3(T0, 128)
    cstb3 = v3(cstb, 128)
    ident_f = cst[:, 0:128]
    tri_f = cst[:, 128:256]
    ones_f = cst[:, 256:384]
    ident_b = cstb[:, 0:128]
    tri_b = cstb[:, 128:256]
    ones_b = cstb[:, 256:384]
    sut_b = cstb[:, 512:640]
    hT3, qT3, attnT3, sg3, G13, GT3 = (v3(U[i], 512) for i in range(6))
    yT3 = [v3(U[1], 512), v3(U[2], 512)]
    bias8v = bias8[:].rearrange("p (c k r f) -> p c k r f", c=2, k=2, r=2)

    def fk(i):
        return ["F%d_0" % i, "F%d_1" % i]

    def mark(name):
        if stop == name:
            raise _Stop()

    rot_state = [0]
    breg = [None]

    def rot():
        rot_state[0] = (rot_state[0] + 1) % 3
        return 2 + rot_state[0]

    def dma_sp(out, in_, r, w, waw=True):
        S.op("sp", lambda e: e.dma_start(out=out, in_=in_), r=r, w=w, waw=waw)

    def dma_pool(out, in_, r, w):
        S.op("pool", lambda e: e.dma_start(out=out, in_=in_), r=r, w=w)

    wlist = []
    for st in range(nst):
        for gi in range(NMIX):
            wlist.append(wall_d[gi])
    for gi in range(3 * nexp):
        wlist.append(wexp_d[gi])
    wstate = {"issued": 0, "next": 0}

    def wget(hold=0):
        n = wstate["next"]
        wstate["next"] += 1
        while wstate["issued"] < min(len(wlist), n + RING - hold):
            i = wstate["issued"]
            src = wlist[i].rearrange("p (a b) -> p a b", b=1024)
            dst = wbuf[i % RING][:].rearrange("p (a b) -> p a b", b=1024)
            dma_pool(dst, src, r=[], w=["wb%d" % (i % RING)])
            wstate["issued"] += 1
        return wbuf[n % RING], "wb%d" % (n % RING)

    try:
        dma_sp(cst[:], cst_d[:, :], [], ["cst"])
        S.dve(lambda e: e.tensor_copy(out=cstb[:], in_=cst[:]), r=["cst"], w=["cstb"])
        for i in range(4):
            dma_sp(lbc[i][:], lnbc_d[i:i + 1, :].to_broadcast([P, D]), [], ["lbc%d" % i])
        dma_sp(lncol[:], lncol_d[:, :], [], ["lncol"])
        dma_sp(maskT[:], maskT_d[:, :], [], ["maskT"])
        dma_sp(convw[:], convw_d[:, :], [], ["convw"])
        dma_sp(convb[:], convb_d[:, :], [], ["convb"])
        dma_sp(bgate[:], bgate_d[:, :], [], ["bgate"])
        dma_sp(wr[:], wr_d[:, :], [], ["wr"])
        dma_sp(ebase[:], ebase_d[:, :], [], ["ebase"])
        for i in range(4):
            dma_sp(smallbc[:, i, :], small_d[i:i + 1, :].to_broadcast([P, 32]), [], ["smallbc"], waw=False)
        S.act(lambda e: e.activation(out=esink[:], in_=smallbc[:, 0, 0:16], func=AF.Exp), r=["smallbc"], w=["esink"])
        S.act(lambda e: e.activation(out=abc[:], in_=smallbc[:, 2, :], func=AF.Exp), r=["smallbc"], w=["abc"])
        S.dve(lambda e: e.tensor_scalar(out=abc[:], in0=abc[:], scalar1=-1.0, scalar2=None, op0=ALU.mult),
              r=["abc"], w=["abc"])
        mask3 = maskT[:].rearrange("p (c q) -> p c q", c=2)
        for c in range(2):
            for kv in range(2):
                ft = Fp[(c * 2 + kv) % 2]
                fkk = fk((c * 2 + kv) % 2)
                off = (c * 2 + kv) * 1024
                dma_sp(ft[:], biasT_d[:, off:off + 1024], [], fkk)
                S.dve(lambda e, ft=ft, c=c, off=off: e.scalar_tensor_tensor(
                    out=bias8[:, off:off + 1024].rearrange("p (h q) -> p h q", q=128),
                    in0=ft[:].rearrange("p (h q) -> p h q", q=128), scalar=8.0,
                    in1=mask3[:, c:c + 1, :].to_broadcast([P, 8, 128]), op0=ALU.mult, op1=ALU.add),
                    r=fkk + ["maskT"], w=["bias8"])
        S.dve(lambda e: e.memset(v_aug[:], 1.0), r=[], w=["vaug"])
        S.dve(lambda e: e.memset(U[5][:], 0.0), r=[], w=["U5"])
        for zi in range(NSLOT // 512):
            dma_sp(XG_d[zi * 512:(zi + 1) * 512, :].rearrange("(p a) d -> p (a d)", a=4), U[5][:], ["U5"], ["XGz"],
                   waw=False)
        S.dve(lambda e: e.memset(base[:], 0.0), r=[], w=["base"])

        mark("setup")
        def layernorm_stats(src, srck):
            S.dve(lambda e: e.bn_stats(out=st6[:, 0:6], in_=src[:, 0:512]), r=srck, w=["st6a"])
            S.dve(lambda e: e.bn_stats(out=st6[:, 6:12], in_=src[:, 512:1024]), r=srck, w=["st6b"])
            S.dve(lambda e: e.bn_aggr(out=mv[:], in_=st6[:]), r=["st6a", "st6b"], w=["mv"])
            S.dve(lambda e: e.tensor_scalar(out=rs[:], in0=mv[:, 1:2], scalar1=EPS, scalar2=None, op0=ALU.add),
                  r=["mv"], w=["rs"])
            S.act(lambda e: e.activation(out=rs[:], in_=rs[:], func=AF.Sqrt), r=["rs"], w=["rs"])
            S.dve(lambda e: e.reciprocal(out=rstd[:], in_=rs[:]), r=["rs"], w=["rstd"])
            S.dve(lambda e: e.tensor_scalar(out=nmr[:], in0=mv[:, 0:1], scalar1=rstd[:, 0:1], scalar2=-1.0,
                                            op0=ALU.mult, op1=ALU.mult), r=["mv", "rstd"], w=["nmr"])

        def normalize(dst, dstk, src, srck):
            S.act(lambda e: e.activation(out=dst, in_=src, func=AF.Identity, scale=rstd[:, 0:1], bias=nmr[:, 0:1]),
                  r=srck + ["rstd", "nmr"], w=dstk)

        def fm_chunk(bank, wview, wk, rhs3, rk, KC, ncols=512):
            bk = "B%d" % bank

            def f(e):
                ins = None
                for kc in range(KC):
                    ins = e.matmul(Bk[bank][:, 0:ncols], lhsT=wview[:, kc, :], rhs=rhs3(kc),
                                   start=(kc == 0), stop=(kc == KC - 1))
                return ins
            S.pe(f, r=[wk] + rk, w=[bk])
            return bk

        for st in range(nst):
            sti = st % 4
            tok0 = st * 512
            if sti == 0:
                S.dve(lambda e: e.memset(state_f[:], 0.0), r=[], w=["statef"])
                S.dve(lambda e: e.memset(state_b[:], 0.0), r=[], w=["stateb"])
                S.dve(lambda e: e.memset(hist[:], 0.0), r=[], w=["hist"])

            for j in range(4):
                xt, xk = Fp[j % 2], fk(j % 2)
                xn, xnk = Fp[2 + j % 2], fk(2 + j % 2)
                dma_sp(xt[:], x_d[tok0 + j * P: tok0 + (j + 1) * P, :], [], xk)
                layernorm_stats(xt, xk)
                normalize(xn[:], xnk, xt[:], xk)
                for hb in range(2):
                    def f(e, xn=xn, hb=hb):
                        ins = None
                        for q in range(4):
                            kc = hb * 4 + q
                            ins = e.transpose(out=Bk[hb][:, q * 128:(q + 1) * 128], in_=xn[:, kc * 128:(kc + 1) * 128],
                                              identity=ident_f)
                        return ins
                    S.pe(f, r=xnk + ["cst"], w=["B%d" % hb])
                    for q in range(4):
                        kc = hb * 4 + q
                        S.act(lambda e, hb=hb, q=q, kc=kc, j=j: e.activation(
                            out=hT3[:, kc, j * 128:(j + 1) * 128], in_=Bk[hb][:, q * 128:(q + 1) * 128],
                            func=AF.Identity, scale=lncol[:, kc:kc + 1], bias=lncol[:, 8 + kc:9 + kc]),
                            r=["B%d" % hb, "lncol"], w=["U0"])

            if st == 0:
                dump_b(0, U[0], "U0")
            mark("stage0")
            for gi in range(3):
                wb, wk = wget()
                mark("A1w")
                w4 = wb[:].rearrange("p (j k c) -> p j k c", j=4, k=8)
                nch = 4 if gi < 2 else 2
                for jj in range(nch):
                    bank = rot()
                    bk = fm_chunk(bank, w4[:, jj], wk, lambda kc: hT3[:, kc, :], ["U0"], 8)
                    mark("A1m")
                    if gi < 2:
                        c = gi * 4 + jj
                        S.act(lambda e, bank=bank, c=c: e.copy(out=qT3[:, c, :], in_=Bk[bank][:]), r=[bk], w=["U1"])
                        mark("A1c")
                    else:
                        S.act(lambda e, bank=bank, jj=jj: e.copy(out=kT[:, jj, 128:640], in_=Bk[bank][:]),
                              r=[bk], w=["kT"])
                        mark("A1k")
                mark("A1g%d" % gi)
            if st == 0:
                dump_b(1, U[1], "U1")
            mark("A1")
            wb, wk = wget()
            w3 = wb[:].rearrange("p (k c) -> p k c", k=8)
            for j in range(4):
                bank = rot()
                bk = "B%d" % bank

                def f(e, bank=bank, j=j, w3=w3):
                    ins = None
                    for kc in range(8):
                        ins = e.matmul(Bk[bank][:, 0:160], lhsT=hT3[:, kc, j * 128:(j + 1) * 128], rhs=w3[:, kc, 0:160],
                                       start=(kc == 0), stop=(kc == 7))
                    return ins
                S.pe(f, r=[wk, "U0"], w=[bk])
                mark("A2m")
                S.act(lambda e, bank=bank, j=j: e.copy(
                    out=v_aug[:, j + 1, :, 0:64], in_=Bk[bank][:, 0:128].rearrange("p (k d) -> p k d", k=2)),
                    r=[bk], w=["vaug"])
                mark("A2v")
                S.dve(lambda e, bank=bank, j=j: e.tensor_tensor(out=dt_t[:, j, :], in0=Bk[bank][:, 128:160],
                                                                in1=smallbc[:, 1, :], op=ALU.add),
                      r=[bk, "smallbc"], w=["dt"])
                mark("A2t")
            mark("A2a")
            S.act(lambda e: e.activation(out=dt_t[:], in_=dt_t[:], func=AF.Exp), r=["dt"], w=["dt"])
            mark("A2b")
            S.dve(lambda e: e.tensor_scalar(out=dt_t[:], in0=dt_t[:], scalar1=1.0, scalar2=None, op0=ALU.add),
                  r=["dt"], w=["dt"])
            S.act(lambda e: e.activation(out=dt_t[:], in_=dt_t[:], func=AF.Ln), r=["dt"], w=["dt"])
            mark("A2d")
            S.dve(lambda e: e.tensor_tensor(out=da_t[:], in0=dt_t[:], in1=abc[:, None, :].to_broadcast([P, 4, 32]),
                                            op=ALU.mult), r=["dt", "abc"], w=["da"])

            mark("A2")
            SC = [[0, 1], [5, 6]]
            for j in range(4):
                sblk = sti * 4 + j
                chunks = [1] if sblk == 0 else [0, 1]
                for kv in range(2):
                    for ch in chunks:
                        koff = (j + ch) * 128
                        for par in range(2):
                            bank = SC[ch][par]
                            bk = "B%d" % bank
                            lo = par * 64
                            S.pe(lambda e, bank=bank, lo=lo, kv=kv, koff=koff, j=j: e.matmul(
                                Bk[bank][:], lhsT=kT[lo:lo + 64, kv, koff:koff + 128],
                                rhs=qT3[lo:lo + 64, kv * 4:(kv + 1) * 4, j * 128:(j + 1) * 128], start=True, stop=True),
                                r=["kT", "U1"], w=[bk])
                            sbt = Fp[4 + ch][:, par * 512:(par + 1) * 512]
                            sbk = "F%d_%d" % (4 + ch, par)
                            S.dve(lambda e, bank=bank, sbt=sbt, ch=ch, kv=kv, par=par: e.tensor_tensor(
                                out=sbt, in0=Bk[bank][:], in1=bias8v[:, ch, kv, par, :], op=ALU.add),
                                r=[bk, "bias8"], w=[sbk])
                            S.act(lambda e, sbt=sbt, ch=ch, par=par: e.activation(
                                out=PT[:, ch, par, :], in_=sbt, func=AF.Exp, scale=0.125), r=[sbk], w=["PT"])

                    def fpv(e, chunks=chunks, kv=kv, j=j):
                        ins = None
                        for h8 in range(8):
                            par, i = h8 % 2, h8 // 2
                            ob = Bk[2 + h8 // 4]
                            o0 = (h8 % 4) * 128
                            for ci, ch in enumerate(chunks):
                                ins = e.matmul(ob[:, o0:o0 + 65], lhsT=PT[:, ch, par, i * 128:(i + 1) * 128],
                                               rhs=v_aug[:, j + ch, kv, 0:65], start=(ci == 0),
                                               stop=(ci == len(chunks) - 1))
                        return ins
                    S.pe(fpv, r=["PT", "vaug"], w=["B2", "B3"])
                    for b in range(2):
                        pv3 = Bk[2 + b][:].rearrange("p (h d) -> p h d", d=128)
                        S.dve(lambda e, b=b, pv3=pv3, kv=kv: e.tensor_tensor(
                            out=den[:, b * 4:(b + 1) * 4], in0=pv3[:, :, 64],
                            in1=esink[:, kv * 8 + b * 4: kv * 8 + b * 4 + 4], op=ALU.add),
                            r=["B%d" % (2 + b), "esink"], w=["den%d" % b])
                    S.dve(lambda e: e.reciprocal(out=rden[:], in_=den[:]), r=["den0", "den1"], w=["rden"])
                    at3 = attn_tok[:].rearrange("p (h d) -> p h d", d=64)
                    for b in range(2):
                        pv3 = Bk[2 + b][:].rearrange("p (h d) -> p h d", d=128)
                        S.dve(lambda e, b=b, pv3=pv3: e.tensor_tensor(
                            out=at3[:, b * 4:(b + 1) * 4, :], in0=pv3[:, :, 0:64],
                            in1=rden[:, b * 4:(b + 1) * 4].to_broadcast([P, 4, 64]), op=ALU.mult),
                            r=["B%d" % (2 + b), "rden"], w=["attntok%d" % b])

                    def ftr(e):
                        ins = None
                        for i in range(4):
                            ins = e.transpose(out=T0v[:, i, :], in_=attn_tok[:, i * 128:(i + 1) * 128], identity=ident_b)
                        return ins
                    S.pe(ftr, r=["attntok0", "attntok1", "cstb"], w=["T0"])
                    S.act(lambda e, kv=kv, j=j: e.copy(out=attnT3[:, kv * 4:(kv + 1) * 4, j * 128:(j + 1) * 128],
                                                       in_=T0v[:, 0:4, :]), r=["T0"], w=["U2"])
            S.dve(lambda e: e.tensor_copy(out=kT[:, :, 0:128], in_=kT[:, :, 512:640]), r=["kT"], w=["kT"])
            S.dve(lambda e: e.tensor_copy(out=v_aug[:, 0], in_=v_aug[:, 4]), r=["vaug"], w=["vaug"])

            if st == 0:
                dump_b(2, U[2], "U2")
            mark("attn")
            for gi in range(2):
                wb, wk = wget()
                w4 = wb[:].rearrange("p (j k c) -> p j k c", j=4, k=8)
                for jj in range(4):
                    c = gi * 4 + jj
                    bank = rot()
                    bk = fm_chunk(bank, w4[:, jj], wk, lambda kc: hT3[:, kc, :], ["U0"], 8)
                    S.act(lambda e, bank=bank, c=c: e.activation(out=sg3[:, c, :], in_=Bk[bank][:], func=AF.Sigmoid,
                                                                 bias=bgate[:, c:c + 1], scale=1.0),
                          r=[bk, "bgate"], w=["U3"])
            for gi in range(2):
                wb, wk = wget()
                w4 = wb[:].rearrange("p (j k c) -> p j k c", j=4, k=8)
                for jj in range(4):
                    c = gi * 4 + jj
                    bank = rot()
                    bk = fm_chunk(bank, w4[:, jj], wk, lambda kc: attnT3[:, kc, :], ["U2"], 8)
                    S.dve(lambda e, bank=bank, c=c: e.tensor_tensor(out=G13[:, c, :], in0=Bk[bank][:], in1=sg3[:, c, :],
                                                                    op=ALU.mult), r=[bk, "U3"], w=["U4"])

            if st == 0:
                dump_b(3, U[3], "U3")
                dump_b(4, U[4], "U4")
            mark("A3")
            for g in range(4):
                dma_sp(ng[:], normg_d[0:1, g * 512:(g + 1) * 512].to_broadcast([P, 512]), [], ["ng"])
                S.dve(lambda e, g=g: e.tensor_tensor(
                    out=Dm[:], in0=cstb3[:, 0:1, :].to_broadcast([P, 8, 128]),
                    in1=smallbc[:, 3, g * 8:(g + 1) * 8].to_broadcast([P, 8, 128]), op=ALU.mult),
                    r=["cstb", "smallbc"], w=["Dm"])
                for gi in range(2):
                    wb, wk = wget()
                    w4 = wb[:].rearrange("p (j k c) -> p j k c", j=4, k=8)
                    nch = 4 if gi == 0 else 2
                    for jj in range(nch):
                        if gi == 0:
                            cc = g * 4 + jj
                            dest, dk = xsT_g[:, jj, :], "xsTg"
                        elif jj == 0:
                            cc = 16 + g
                            dest, dk = BT_g[:], "BTg"
                        else:
                            cc = 20 + g
                            dest, dk = CT_g[:], "CTg"
                        bank = rot()
                        bk = fm_chunk(bank, w4[:, jj], wk, lambda kc: hT3[:, kc, :], ["U0"], 8)
                        ri = cc % 2
                        raw, rk = Fp[ri], fk(ri)
                        ta, tk = Fp[2][:, ri * 512:(ri + 1) * 512], "F2_%d" % ri
                        S.dve(lambda e, raw=raw, cc=cc: e.tensor_copy(out=raw[:, 0:3], in_=hist[:, cc, :]),
                              r=["hist"], w=rk)
                        S.act(lambda e, raw=raw, bank=bank: e.copy(out=raw[:, 3:515], in_=Bk[bank][:]),
                              r=[bk], w=rk)
                        S.dve(lambda e, raw=raw, ta=ta, cc=cc: e.tensor_scalar(
                            out=ta, in0=raw[:, 0:512], scalar1=convw[:, cc * 4:cc * 4 + 1], scalar2=convb[:, cc:cc + 1],
                            op0=ALU.mult, op1=ALU.add), r=rk + ["convw", "convb"], w=[tk])
                        for tap in range(1, 4):
                            S.dve(lambda e, raw=raw, ta=ta, cc=cc, tap=tap: e.scalar_tensor_tensor(
                                out=ta, in0=raw[:, tap:tap + 512], scalar=convw[:, cc * 4 + tap:cc * 4 + tap + 1],
                                in1=ta, op0=ALU.mult, op1=ALU.add), r=rk + [tk, "convw"], w=[tk])
                        S.dve(lambda e, raw=raw, cc=cc: e.tensor_copy(out=hist[:, cc, :], in_=raw[:, 512:515]),
                              r=rk, w=["hist"])
                        S.act(lambda e, ta=ta, dest=dest: e.activation(out=dest, in_=ta, func=AF.Silu), r=[tk], w=[dk])
                for j in range(4):
                    def ftx(e, j=j):
                        ins = None
                        for c in range(4):
                            ins = e.transpose(out=T0v[:, c, :], in_=xsT_g[:, c, j * 128:(j + 1) * 128], identity=ident_b)
                        ins = e.transpose(out=T0v[:, 4, :], in_=BT_g[:, j * 128:(j + 1) * 128], identity=ident_b)
                        return ins
                    S.pe(ftx, r=["xsTg", "BTg", "cstb"], w=["T0"])
                    S.act(lambda e, j=j: e.copy(out=xsB[:, j, :], in_=T0[:, 0:640]), r=["T0"], w=["xsB"])
                wb, wk = wget()
                w3 = wb[:].rearrange("p (k c) -> p k c", k=8)
                for j in range(4):
                    bank = rot()
                    bk = "B%d" % bank

                    def fz(e, bank=bank, j=j, w3=w3):
                        ins = None
                        for kc in range(8):
                            ins = e.matmul(Bk[bank][:], lhsT=hT3[:, kc, j * 128:(j + 1) * 128], rhs=w3[:, kc, :],
                                           start=(kc == 0), stop=(kc == 7))
                        return ins
                    S.pe(fz, r=[wk, "U0"], w=[bk])
                    S.act(lambda e, bank=bank, j=j: e.activation(out=sz[:, j, :], in_=Bk[bank][:], func=AF.Silu),
                          r=[bk], w=["sz"])
                for j in range(4):
                    jsl = slice(j * 128, (j + 1) * 128)
                    dts = dt_t[:, j, g * 8:(g + 1) * 8]
                    das = da_t[:, j, g * 8:(g + 1) * 8]
                    xs3 = xsB[:, j, 0:512].rearrange("p (h d) -> p h d", d=64)
                    S.dve(lambda e, das=das: e.tensor_copy(out=dab[:], in_=das), r=["da"], w=["dab"])
                    S.dve(lambda e: e.tensor_tensor(out=Whi[:], in0=cstb3[:, 3:4, :].to_broadcast([P, 8, 128]),
                                                    in1=dab[:].to_broadcast([P, 8, 128]), op=ALU.mult),
                          r=["cstb", "dab"], w=["Whi"])

                    def fseg(e):
                        ins = None
                        for h in range(8):
                            ins = e.matmul(Bk[h // 4][:, (h % 4) * 128:(h % 4 + 1) * 128], lhsT=Whi[:, h, :], rhs=tri_b,
                                           start=True, stop=True)
                        return ins
                    S.pe(fseg, r=["Whi", "cstb"], w=["B0", "B1"])
                    for b in range(2):
                        S.act(lambda e, b=b: e.activation(out=Bk[b][:], in_=Bk[b][:], func=AF.Exp),
                              r=["B%d" % b], w=["B%d" % b])
                    S.pe(lambda e, jsl=jsl: e.matmul(Bk[5][:, 0:128], lhsT=BT_g[:, jsl], rhs=CT_g[:, jsl],
                                                     start=True, stop=True), r=["BTg", "CTg"], w=["B5"])
                    S.dve(lambda e: e.tensor_tensor(out=CBm[:, 0, :], in0=Bk[5][:, 0:128], in1=tri_f, op=ALU.mult),
                          r=["B5", "cst"], w=["CBm"])
                    for b in range(2):
                        sg_ = Bk[b][:].rearrange("p (h t) -> p h t", t=128)
                        S.dve(lambda e, b=b, sg_=sg_: e.tensor_tensor(
                            out=MT[:, b * 4:(b + 1) * 4, :], in0=sg_, in1=CBm[:, 0:1, :].to_broadcast([P, 4, 128]),
                            op=ALU.mult), r=["B%d" % b, "CBm"], w=["MT%d" % b])
                        S.dve(lambda e, b=b, sg_=sg_, dts=dts: e.tensor_tensor(
                            out=wst[:, b * 4:(b + 1) * 4], in0=sg_[:, :, 127], in1=dts[:, b * 4:(b + 1) * 4],
                            op=ALU.mult), r=["B%d" % b, "dt"], w=["wst%d" % b])
                    S.dve(lambda e, xs3=xs3, dts=dts: e.tensor_tensor(out=xdt[:], in0=xs3,
                                                                      in1=dts.to_broadcast([P, 8, 64]), op=ALU.mult),
                          r=["xsB", "dt"], w=["xdt"])
                    S.dve(lambda e, xs3=xs3: e.tensor_tensor(out=xw[:], in0=xs3, in1=wst[:].to_broadcast([P, 8, 64]),
                                                             op=ALU.mult), r=["xsB", "wst0", "wst1"], w=["xw"])

                    def fy(e, xs3=xs3):
                        ins = None
                        for h in range(8):
                            e.matmul(Bk[2][:, h * 64:(h + 1) * 64], lhsT=MT[:, h, :], rhs=xdt[:, h, :],
                                     start=True, stop=False)
                            ins = e.matmul(Bk[2][:, h * 64:(h + 1) * 64], lhsT=Dm[:, h, :], rhs=xs3[:, h, :],
                                           start=False, stop=True)
                        return ins
                    S.pe(fy, r=["MT0", "MT1", "xdt", "Dm", "xsB"], w=["B2"])
                    S.pe(lambda e, jsl=jsl, g=g: e.matmul(Bk[3][:], lhsT=CT_g[:, jsl], rhs=state_b[:, g, :],
                                                          start=True, stop=True), r=["CTg", "stateb"], w=["B3"])

                    def fal(e, das=das):
                        e.matmul(Bk[6][:, 0:8], lhsT=tri_f, rhs=das, start=True, stop=True)
                        return e.matmul(Bk[6][:, 8:16], lhsT=ones_f, rhs=das, start=True, stop=True)
                    S.pe(fal, r=["da", "cst"], w=["B6"])
                    S.act(lambda e: e.activation(out=eac[:], in_=Bk[6][:, 0:16], func=AF.Exp), r=["B6"], w=["eac"])
                    y1 = Fp[3][:, 0:512]
                    y2 = Fp[3][:, 512:1024]
                    yz = Fp[4][:, 0:512]
                    stmp = Fp[4][:, 512:1024]
                    S.dve(lambda e, y1=y1: e.tensor_tensor(
                        out=y1.rearrange("p (h d) -> p h d", d=64), in0=Bk[3][:].rearrange("p (h d) -> p h d", d=64),
                        in1=eac[:, 0:8].to_broadcast([P, 8, 64]), op=ALU.mult), r=["B3", "eac"], w=["F3_0"])
                    S.dve(lambda e, y1=y1, y2=y2: e.tensor_tensor(out=y2, in0=Bk[2][:], in1=y1, op=ALU.add),
                          r=["B2", "F3_0"], w=["F3_1"])
                    S.dve(lambda e, y2=y2, yz=yz, j=j: e.tensor_tensor(out=yz, in0=y2, in1=sz[:, j, :], op=ALU.mult),
                          r=["F3_1", "sz"], w=["F4_0"])
                    S.act(lambda e, yz=yz, y1=y1: e.activation(out=y1, in_=yz, func=AF.Square, accum_out=ss[:]),
                          r=["F4_0"], w=["F3_0", "ss"])
                    S.dve(lambda e: e.tensor_scalar(out=rs[:], in0=ss[:], scalar1=1.0 / 512.0, scalar2=EPS,
                                                    op0=ALU.mult, op1=ALU.add), r=["ss"], w=["rs"])
                    S.act(lambda e: e.activation(out=rs[:], in_=rs[:], func=AF.Sqrt), r=["rs"], w=["rs"])
                    S.dve(lambda e: e.reciprocal(out=rstd[:], in_=rs[:]), r=["rs"], w=["rstd"])
                    S.dve(lambda e, yz=yz: e.scalar_tensor_tensor(out=yn[:], in0=yz, scalar=rstd[:, 0:1], in1=ng[:],
                                                                  op0=ALU.mult, op1=ALU.mult),
                          r=["F4_0", "rstd", "ng"], w=["yn"])

                    def fty(e):
                        ins = None
                        for c in range(4):
                            ins = e.transpose(out=T0v[:, c, :], in_=yn[:, c * 128:(c + 1) * 128], identity=ident_b)
                        return ins
                    S.pe(fty, r=["yn", "cstb"], w=["T0"])
                    yt = yT3[g // 2]
                    lc = (g % 2) * 4
                    S.act(lambda e, yt=yt, lc=lc, jsl=jsl: e.copy(out=yt[:, lc:lc + 4, jsl], in_=T0v[:, 0:4, :]),
                          r=["T0"], w=["U%d" % (1 + g // 2)])
                    S.pe(lambda e, j=j: e.matmul(Bk[4][:], lhsT=xsB[:, j, 512:640], rhs=xw[:].rearrange("p h d -> p (h d)"),
                                                 start=True, stop=True), r=["xsB", "xw"], w=["B4"])
                    S.dve(lambda e, g=g, stmp=stmp: e.tensor_tensor(
                        out=stmp.rearrange("p (h d) -> p h d", d=64),
                        in0=state_f[:, g, :].rearrange("p (h d) -> p h d", d=64),
                        in1=eac[:, 8:16].to_broadcast([P, 8, 64]), op=ALU.mult), r=["statef", "eac"], w=["F4_1"])
                    S.dve(lambda e, g=g, stmp=stmp: e.tensor_tensor(out=state_f[:, g, :], in0=Bk[4][:], in1=stmp,
                                                                    op=ALU.add), r=["B4", "F4_1"], w=["statef"])
                    S.act(lambda e, g=g: e.copy(out=state_b[:, g, :], in_=state_f[:, g, :]), r=["statef"], w=["stateb"])

            if st == 0:
                dump_b(5, U[1], "U1")
                dump_b(6, U[2], "U2")
            mark("C")
            for gi in range(2):
                wb, wk = wget()
                w4 = wb[:].rearrange("p (j k c) -> p j k c", j=4, k=8)
                for jj in range(4):
                    c = gi * 4 + jj
                    bank = rot()
                    bk = fm_chunk(bank, w4[:, jj], wk, lambda kc: hT3[:, kc, :], ["U0"], 8)
                    S.act(lambda e, bank=bank, c=c: e.activation(out=sg3[:, c, :], in_=Bk[bank][:], func=AF.Sigmoid,
                                                                 bias=bgate[:, 8 + c:9 + c], scale=1.0),
                          r=[bk, "bgate"], w=["U3"])
            for gi in range(4):
                wb, wk = wget()
                w4 = wb[:].rearrange("p (j k c) -> p j k c", j=2, k=16)
                for jj in range(2):
                    c = gi * 2 + jj
                    bank = rot()
                    bk = fm_chunk(bank, w4[:, jj], wk, lambda kc: yT3[kc // 8][:, kc % 8, :], ["U1", "U2"], 16)
                    tg = Fp[3][:, 0:512]
                    S.dve(lambda e, bank=bank, c=c, tg=tg: e.tensor_tensor(out=tg, in0=Bk[bank][:], in1=sg3[:, c, :],
                                                                           op=ALU.mult), r=[bk, "U3"], w=["F3_0"])
                    S.dve(lambda e, c=c, tg=tg: e.tensor_tensor(out=GT3[:, c, :], in0=tg, in1=G13[:, c, :], op=ALU.add),
                          r=["F3_0", "U4"], w=["U5"])

            if st == 0:
                dump_b(7, U[5], "U5")
            mark("D")
            wo = []
            for hf in range(2):
                wb, wk = wget(hold=hf)
                wo.append((wb[:].rearrange("p (k c) -> p k c", k=8), wk))
            for j in range(4):
                t = st * 4 + j
                xt, xk = Fp[j % 2], fk(j % 2)
                xn, xnk = Fp[2 + j % 2], fk(2 + j % 2)
                rr = Fp[4]
                h1, h1k = Fp[5], fk(5)
                dma_sp(xt[:], x_d[tok0 + j * P: tok0 + (j + 1) * P, :], [], xk)
                layernorm_stats(xt, xk)
                normalize(xn[:], xnk, xt[:], xk)
                S.dve(lambda e, xn=xn: e.tensor_tensor(out=xn[:], in0=xn[:], in1=lbc[0][:], op=ALU.mult),
                      r=xnk + ["lbc0"], w=xnk)
                S.dve(lambda e, xn=xn: e.tensor_tensor(out=xn[:], in0=xn[:], in1=lbc[1][:], op=ALU.add),
                      r=xnk + ["lbc1"], w=xnk)
                for hf in range(2):
                    bank = 2 + hf
                    bk = "B%d" % bank
                    w3, wk = wo[hf]

                    def fo(e, bank=bank, j=j, w3=w3):
                        ins = None
                        for kc in range(8):
                            ins = e.matmul(Bk[bank][:], lhsT=GT3[:, kc, j * 128:(j + 1) * 128], rhs=w3[:, kc, :],
                                           start=(kc == 0), stop=(kc == 7))
                        return ins
                    S.pe(fo, r=[wk, "U5"], w=[bk])
                    S.dve(lambda e, bank=bank, hf=hf, xn=xn, rr=rr: e.scalar_tensor_tensor(
                        out=rr[:, hf * 512:(hf + 1) * 512], in0=xn[:, hf * 512:(hf + 1) * 512], scalar=ALPHA,
                        in1=Bk[bank][:], op0=ALU.mult, op1=ALU.add), r=[bk] + xnk, w=["F4_%d" % hf])
                S.dve(lambda e, rr=rr: e.bn_stats(out=st6[:, 0:6], in_=rr[:, 0:512]), r=["F4_0"], w=["st6a"])
                S.dve(lambda e, rr=rr: e.bn_stats(out=st6[:, 6:12], in_=rr[:, 512:1024]), r=["F4_1"], w=["st6b"])
                S.dve(lambda e: e.bn_aggr(out=mv[:], in_=st6[:]), r=["st6a", "st6b"], w=["mv"])
                S.dve(lambda e: e.tensor_scalar(out=rs[:], in0=mv[:, 1:2], scalar1=EPS, scalar2=None, op0=ALU.add),
                      r=["mv"], w=["rs"])
                S.act(lambda e: e.activation(out=rs[:], in_=rs[:], func=AF.Sqrt), r=["rs"], w=["rs"])
                S.dve(lambda e: e.reciprocal(out=rstd[:], in_=rs[:]), r=["rs"], w=["rstd"])
                S.dve(lambda e: e.tensor_scalar(out=nmr[:], in0=mv[:, 0:1], scalar1=rstd[:, 0:1], scalar2=-1.0,
                                                op0=ALU.mult, op1=ALU.mult), r=["mv", "rstd"], w=["nmr"])
                S.act(lambda e, rr=rr, h1=h1: e.activation(out=h1[:], in_=rr[:], func=AF.Identity, scale=rstd[:, 0:1],
                                                           bias=nmr[:, 0:1]),
                      r=["F4_0", "F4_1", "rstd", "nmr"], w=h1k)
                S.dve(lambda e, h1=h1: e.tensor_tensor(out=h1[:], in0=h1[:], in1=lbc[2][:], op=ALU.mult),
                      r=h1k + ["lbc2"], w=h1k)
                S.dve(lambda e, h1=h1: e.tensor_tensor(out=h1[:], in0=h1[:], in1=lbc[3][:], op=ALU.add),
                      r=h1k + ["lbc3"], w=h1k)
                dma_sp(H1_d[t * P:(t + 1) * P, :], h1[:], h1k, [("H1", t)])
                S.act(lambda e, h1=h1: e.copy(out=h1b[:], in_=h1[:]), r=h1k, w=["h1b"])
                for hb in range(2):
                    def ftr2(e, hb=hb, h1=h1):
                        ins = None
                        for q in range(4):
                            kc = hb * 4 + q
                            ins = e.transpose(out=Bk[hb][:, q * 128:(q + 1) * 128], in_=h1[:, kc * 128:(kc + 1) * 128],
                                              identity=ident_f)
                        return ins
                    S.pe(ftr2, r=h1k + ["cst"], w=["B%d" % hb])
                    S.act(lambda e, hb=hb: e.copy(out=h1T[:, hb * 4:(hb + 1) * 4, :],
                                                  in_=Bk[hb][:].rearrange("p (a b) -> p a b", b=128)),
                          r=["B%d" % hb], w=["h1T%d" % hb])
                wr3 = wr[:].rearrange("p (k c) -> p k c", k=8)

                def frt(e):
                    ins = None
                    for kc in range(8):
                        ins = e.matmul(Bk[5][:, 0:36], lhsT=h1T[:, kc, :], rhs=wr3[:, kc, :], start=(kc == 0), stop=(kc == 7))
                    return ins
                S.pe(frt, r=["h1T0", "h1T1", "wr"], w=["B5"])
                S.act(lambda e: e.copy(out=lg[:], in_=Bk[5][:, 0:36]), r=["B5"], w=["lg"])
                gmax = rt[:, 0:1]
                og = rt[:, 4:8]
                gsh = rt[:, 8:12]
                gsum = rt[:, 12:13]
                pg = rt[:, 13:14]
                dd = rt[:, 14:15]
                ed = rt[:, 15:16]
                p1 = rt[:, 16:17]
                dn1 = rt[:, 17:18]
                i1f = rt[:, 18:19]
                i2f = rt[:, 19:20]
                esel = rt[:, 24:32]
                prod = rt[:, 32:64]
                oh1 = rt[:, 64:96]
                oh2 = rt[:, 96:128]
                sel1 = rt[:, 128:136]
                sel2 = rt[:, 136:144]
                prod3 = prod.rearrange("p (g j) -> p g j", j=8)
                oh13 = oh1.rearrange("p (g j) -> p g j", j=8)
                oh23 = oh2.rearrange("p (g j) -> p g j", j=8)
                lg3 = lg[:, 4:36].rearrange("p (g j) -> p g j", j=8)
                R = "rt"
                S.dve(lambda e: e.tensor_reduce(out=gmax, in_=lg[:, 0:4], axis=AX.X, op=ALU.max), r=["lg"], w=[R])
                S.dve(lambda e: e.tensor_scalar(out=og, in0=lg[:, 0:4], scalar1=gmax, scalar2=None, op0=ALU.is_ge),
                      r=["lg", R], w=[R])
                S.dve(lambda e: e.tensor_scalar(out=gsh, in0=lg[:, 0:4], scalar1=gmax, scalar2=None, op0=ALU.subtract),
                      r=["lg", R], w=[R])
                S.act(lambda e: e.activation(out=gsh, in_=gsh, func=AF.Exp, accum_out=gsum), r=[R], w=[R])
                S.dve(lambda e: e.reciprocal(out=pg, in_=gsum), r=[R], w=[R])
                S.dve(lambda e: e.tensor_tensor(out=prod3, in0=lg3, in1=og.to_broadcast([P, 4, 8]), op=ALU.mult),
                      r=["lg", R], w=[R])
                S.dve(lambda e: e.tensor_reduce(out=esel, in_=prod3.rearrange("p g j -> p j g"), axis=AX.X, op=ALU.add),
                      r=[R], w=[R])
                S.dve(lambda e: e.max(out=top8[:], in_=esel), r=[R], w=["top8"])
                S.dve(lambda e: e.tensor_tensor(out=dd, in0=top8[:, 1:2], in1=top8[:, 0:1], op=ALU.subtract),
                      r=["top8"], w=[R])
                S.act(lambda e: e.activation(out=ed, in_=dd, func=AF.Exp), r=[R], w=[R])
                S.dve(lambda e: e.tensor_scalar(out=dn1, in0=ed, scalar1=1.0, scalar2=None, op0=ALU.add), r=[R], w=[R])
                S.dve(lambda e: e.reciprocal(out=p1, in_=dn1), r=[R], w=[R])
                S.dve(lambda e, t=t: e.tensor_tensor(out=w_all[:, t, 0:1], in0=pg, in1=p1, op=ALU.mult),
                      r=[R], w=["wall_s"])
                S.dve(lambda e, t=t: e.tensor_tensor(out=w_all[:, t, 1:2], in0=w_all[:, t, 0:1], in1=ed, op=ALU.mult),
                      r=[R, "wall_s"], w=["wall_s"])
                S.dve(lambda e: e.tensor_scalar(out=sel1, in0=esel, scalar1=top8[:, 0:1], scalar2=None, op0=ALU.is_equal),
                      r=[R, "top8"], w=[R])
                S.dve(lambda e: e.tensor_scalar(out=sel2, in0=esel, scalar1=top8[:, 1:2], scalar2=None, op0=ALU.is_equal),
                      r=[R, "top8"], w=[R])
                S.dve(lambda e: e.tensor_tensor(out=oh13, in0=og.to_broadcast([P, 4, 8]),
                                                in1=sel1[:, None, :].to_broadcast([P, 4, 8]), op=ALU.mult), r=[R], w=[R])
                S.dve(lambda e: e.tensor_tensor(out=oh23, in0=og.to_broadcast([P, 4, 8]),
                                                in1=sel2[:, None, :].to_broadcast([P, 4, 8]), op=ALU.mult), r=[R], w=[R])
                S.dve(lambda e: e.tensor_tensor(out=mb[:], in0=oh1, in1=oh2, op=ALU.add), r=[R], w=["mb"])

                def fcn(e):
                    e.matmul(Bk[6][:, 0:32], lhsT=sut_b, rhs=mb[:], start=True, stop=True)
                    return e.matmul(Bk[6][:, 32:64], lhsT=ones_b, rhs=mb[:], start=True, stop=True)
                S.pe(fcn, r=["mb", "cstb"], w=["B6"])
                S.dve(lambda e: e.tensor_tensor(out=prod, in0=Bk[6][:, 0:32], in1=base[:], op=ALU.add),
                      r=["B6", "base"], w=[R])
                S.dve(lambda e: e.tensor_tensor(out=base[:], in0=Bk[6][:, 32:64], in1=base[:], op=ALU.add),
                      r=["B6", "base"], w=["base"])
                S.dve(lambda e: e.tensor_scalar(out=prod, in0=prod, scalar1=float(CAP - 1), scalar2=None, op0=ALU.min),
                      r=[R], w=[R])
                S.dve(lambda e: e.tensor_tensor(out=prod, in0=prod, in1=ebase[:], op=ALU.add), r=[R, "ebase"], w=[R])
                S.dve(lambda e: e.tensor_tensor(out=oh1, in0=oh1, in1=prod, op=ALU.mult), r=[R], w=[R])
                S.dve(lambda e: e.tensor_tensor(out=oh2, in0=oh2, in1=prod, op=ALU.mult), r=[R], w=[R])
                S.dve(lambda e: e.tensor_reduce(out=i1f, in_=oh1, axis=AX.X, op=ALU.add), r=[R], w=[R])
                S.dve(lambda e: e.tensor_reduce(out=i2f, in_=oh2, axis=AX.X, op=ALU.add), r=[R], w=[R])
                S.dve(lambda e, t=t: e.tensor_copy(out=idx_all[:, t, 0:1], in_=i1f), r=[R], w=["idxall"])
                S.dve(lambda e, t=t: e.tensor_copy(out=idx_all[:, t, 1:2], in_=i2f), r=[R], w=["idxall"])
                for k in range(2):
                    S.op("pool", lambda e, t=t, k=k: e.indirect_dma_start(
                        out=XG_d[:, :], out_offset=bass.IndirectOffsetOnAxis(ap=idx_all[:, t, k:k + 1], axis=0),
                        in_=h1b[:, :], in_offset=None, bounds_check=breg[0], oob_is_err=False),
                        r=["h1b", "idxall", "XGz"], w=["XG"], waw=False)

        mark("E")
        for ex in range(nexp):
            wgt, wgk = wget()
            wup, wuk = wget(hold=1)
            wg4 = wgt[:].rearrange("p (j k c) -> p j k c", j=4, k=8)
            wu4 = wup[:].rearrange("p (j k c) -> p j k c", j=4, k=8)
            hid3 = U[2][:, 0:3072].rearrange("p (f n) -> p f n", f=4)
            for half in range(2):
                row0 = ex * CAP + half * 384
                xgt, xgk = U[0 if half == 0 else 4], "U%d" % (0 if half == 0 else 4)
                xgT, xTk = U[1 if half == 0 else 3], "U%d" % (1 if half == 0 else 3)
                xgt3 = xgt[:, 0:3072].rearrange("p (b d) -> p b d", d=1024)
                xgT3 = xgT[:, 0:3072].rearrange("p (k n) -> p k n", k=8)
                dma_sp(xgt3, XG_d[row0:row0 + 384, :].rearrange("(b p) d -> p b d", p=P), ["XG"], [xgk])
                for b in range(3):
                    def ftg(e, b=b, xgt3=xgt3):
                        ins = None
                        for kc in range(8):
                            ins = e.transpose(out=T0v[:, kc, :], in_=xgt3[:, b, kc * 128:(kc + 1) * 128], identity=ident_b)
                        return ins
                    S.pe(ftg, r=[xgk, "cstb"], w=["T0"])
                    S.act(lambda e, b=b, xgT3=xgT3: e.copy(out=xgT3[:, :, b * 128:(b + 1) * 128], in_=T0v[:, :, :]),
                          r=["T0"], w=[xTk])
                for fc in range(4):
                    bg, bu = fc % 2, 2 + fc % 2

                    def fg(e, bg=bg, fc=fc, xgT3=xgT3):
                        ins = None
                        for kc in range(8):
                            ins = e.matmul(Bk[bg][:, 0:384], lhsT=wg4[:, fc, kc, :], rhs=xgT3[:, kc, :],
                                           start=(kc == 0), stop=(kc == 7))
                        return ins

                    def fu(e, bu=bu, fc=fc, xgT3=xgT3):
                        ins = None
                        for kc in range(8):
                            ins = e.matmul(Bk[bu][:, 0:384], lhsT=wu4[:, fc, kc, :], rhs=xgT3[:, kc, :],
                                           start=(kc == 0), stop=(kc == 7))
                        return ins
                    S.pe(fg, r=[wgk, xTk], w=["B%d" % bg])
                    S.pe(fu, r=[wuk, xTk], w=["B%d" % bu])
                    sgt, sgk = Fp[fc % 2][:, 0:384], "F%d_0" % (fc % 2)
                    S.act(lambda e, bg=bg, sgt=sgt: e.activation(out=sgt, in_=Bk[bg][:, 0:384], func=AF.Silu),
                          r=["B%d" % bg], w=[sgk])
                    S.dve(lambda e, bu=bu, sgt=sgt, fc=fc, half=half: e.tensor_tensor(
                        out=hid3[:, fc, half * 384:(half + 1) * 384], in0=Bk[bu][:, 0:384], in1=sgt, op=ALU.mult),
                        r=["B%d" % bu, sgk], w=["U2"])
            wdn, wdk = wget()
            wd3 = wdn[:].rearrange("p (f c) -> p f c", f=4)
            for b in range(6):
                yg, ygk = Fp[2 + b % 2], fk(2 + b % 2)
                for hf in range(2):
                    bank = 4 + (b * 2 + hf) % 3
                    bk = "B%d" % bank

                    def fd(e, bank=bank, b=b, hf=hf):
                        ins = None
                        for fc in range(4):
                            ins = e.matmul(Bk[bank][:], lhsT=hid3[:, fc, b * 128:(b + 1) * 128],
                                           rhs=wd3[:, fc, hf * 512:(hf + 1) * 512], start=(fc == 0), stop=(fc == 3))
                        return ins
                    S.pe(fd, r=[wdk, "U2"], w=[bk])
                    S.act(lambda e, bank=bank, yg=yg, hf=hf: e.copy(out=yg[:, hf * 512:(hf + 1) * 512], in_=Bk[bank][:]),
                          r=[bk], w=["F%d_%d" % (2 + b % 2, hf)])
                r0 = ex * CAP + b * P
                dma_sp(YG_d[r0:r0 + P, :], yg[:], ygk, ["YG"], waw=False)

        mark("moe")
        dma_sp(lbc[0][:], lnbc_d[4:5, :].to_broadcast([P, D]), [], ["lbc0"])
        dma_sp(lbc[1][:], lnbc_d[5:6, :].to_broadcast([P, D]), [], ["lbc1"])
        for t in range(nst * 4):
            o = (t % 2) * 3
            y1t, y1k = Fp[o], fk(o)
            y2t, y2k = Fp[o + 1], fk(o + 1)
            h1t, hk = Fp[o + 2], fk(o + 2)
            for k, (yt_, yk_) in enumerate(((y1t, y1k), (y2t, y2k))):
                S.op("pool", lambda e, t=t, k=k, yt_=yt_: e.indirect_dma_start(
                    out=yt_[:, :], out_offset=None, in_=YG_d[:, :],
                    in_offset=bass.IndirectOffsetOnAxis(ap=idx_all[:, t, k:k + 1], axis=0),
                    bounds_check=breg[0], oob_is_err=False), r=["YG", "idxall"], w=yk_)
            dma_sp(h1t[:], H1_d[t * P:(t + 1) * P, :], [("H1", t)], hk)
            S.act(lambda e, h1t=h1t: e.activation(out=h1t[:], in_=h1t[:], func=AF.Copy, scale=ALPHA), r=hk, w=hk)
            S.dve(lambda e, h1t=h1t, y1t=y1t, t=t: e.scalar_tensor_tensor(
                out=h1t[:], in0=y1t[:], scalar=w_all[:, t, 0:1], in1=h1t[:], op0=ALU.mult, op1=ALU.add),
                r=y1k + hk + ["wall_s"], w=hk)
            S.dve(lambda e, h1t=h1t, y2t=y2t, t=t: e.scalar_tensor_tensor(
                out=h1t[:], in0=y2t[:], scalar=w_all[:, t, 1:2], in1=h1t[:], op0=ALU.mult, op1=ALU.add),
                r=y2k + hk + ["wall_s"], w=hk)
            layernorm_stats(h1t, hk)
            normalize(y1t[:], y1k, h1t[:], hk)
            S.dve(lambda e, y1t=y1t: e.tensor_tensor(out=y1t[:], in0=y1t[:], in1=lbc[0][:], op=ALU.mult),
                  r=y1k + ["lbc0"], w=y1k)
            S.dve(lambda e, y1t=y1t: e.tensor_tensor(out=y1t[:], in0=y1t[:], in1=lbc[1][:], op=ALU.add),
                  r=y1k + ["lbc1"], w=y1k)
            dma_sp(out_d[t * P:(t + 1) * P, :], y1t[:], y1k, [("out", t)])


    except _Stop:
        pass
    S.finish()

    with nc.Block() as block:
        @block.tensor
        def _(e):
            for f in S.ops["pe"]:
                f(e)

        @block.scalar
        def _(e):
            for f in S.ops["act"]:
                f(e)

        @block.vector
        def _(e):
            for f in S.ops["dve"]:
                f(e)

        @block.gpsimd
        def _(e):
            breg[0] = e.to_reg(NSLOT - 1)
            for f in S.ops["pool"]:
                f(e)

        @block.sync
        def _(e):
            for f in S.ops["sp"]:
                f(e)
    es.close()
    return nc


def _t5_bucket(dist):
    max_exact = 16
    d_f = np.maximum(dist, 1).astype(np.float32)
    large = max_exact + (np.log(d_f / max_exact) / math.log(128 / max_exact) * (32 - max_exact)).astype(np.int32)
    large = np.minimum(large, 31)
    return np.where(dist < max_exact, dist, large)


def _fm_group(W, chunks, kc=8):
    nj = len(chunks)
    out = np.zeros((P, nj, kc, 128), np.float32)
    for j, cols in enumerate(chunks):
        if cols is None:
            continue
        blk = W[:, cols]
        out[:, j] = blk.reshape(kc, P, 128).transpose(1, 0, 2)
    return out.reshape(P, 4096)


def _tm_group(W, cols):
    out = np.zeros((P, 8, 512), np.float32)
    blk = W[:, cols]
    out[:, :, :len(cols)] = blk.reshape(8, P, len(cols)).transpose(1, 0, 2)
    return out.reshape(P, 4096)


def _prep_shared(inp):
    f32 = np.float32
    w_in = np.asarray(inp["w_in"][0], f32)
    ar = np.arange(128)
    groups = []
    groups.append(_fm_group(w_in, [OFF_Q + c * 128 + ar for c in range(4)]))
    groups.append(_fm_group(w_in, [OFF_Q + c * 128 + ar for c in range(4, 8)]))
    k0 = OFF_K + np.concatenate([np.arange(64), np.arange(64)])
    k1 = OFF_K + 64 + np.concatenate([np.arange(64), np.arange(64)])
    groups.append(_fm_group(w_in, [k0, k1, None, None]))
    groups.append(_tm_group(w_in, np.concatenate([OFF_V + ar, OFF_DT + np.arange(32)])))
    groups.append(_fm_group(w_in, [OFF_GA + c * 128 + ar for c in range(4)]))
    groups.append(_fm_group(w_in, [OFF_GA + c * 128 + ar for c in range(4, 8)]))
    w_ao = np.asarray(inp["w_attn_out"][0], f32)
    groups.append(_fm_group(w_ao, [c * 128 + ar for c in range(4)]))
    groups.append(_fm_group(w_ao, [c * 128 + ar for c in range(4, 8)]))
    for g in range(4):
        groups.append(_fm_group(w_in, [OFF_XS + g * 512 + c * 128 + ar for c in range(4)]))
        groups.append(_fm_group(w_in, [OFF_B + g * 128 + ar, OFF_C + g * 128 + ar, None, None]))
        groups.append(_tm_group(w_in, OFF_Z + g * 512 + np.arange(512)))
    groups.append(_fm_group(w_in, [OFF_GS + c * 128 + ar for c in range(4)]))
    groups.append(_fm_group(w_in, [OFF_GS + c * 128 + ar for c in range(4, 8)]))
    w_so = np.asarray(inp["w_ssm_out"][0], f32)
    for gi in range(4):
        groups.append(_fm_group(w_so, [(gi * 2 + jj) * 128 + ar for jj in range(2)], kc=16))
    w_out = np.asarray(inp["w_out"][0], f32)
    groups.append(_tm_group(w_out, np.arange(512)))
    groups.append(_tm_group(w_out, 512 + np.arange(512)))
    wall = np.stack(groups)
    assert wall.shape[0] == NMIX

    wge = np.asarray(inp["w_gate_e"][0], f32)
    wue = np.asarray(inp["w_up_e"][0], f32)
    wde = np.asarray(inp["w_down_e"][0], f32)
    wexp = np.zeros((96, P, 4096), f32)
    for e in range(32):
        wexp[3 * e] = wge[e].reshape(8, P, 4, 128).transpose(1, 2, 0, 3).reshape(P, 4096)
        wexp[3 * e + 1] = wue[e].reshape(8, P, 4, 128).transpose(1, 2, 0, 3).reshape(P, 4096)
        wexp[3 * e + 2] = wde[e].reshape(4, P, 1024).transpose(1, 0, 2).reshape(P, 4096)

    k = np.arange(128)[:, None]
    t = np.arange(128)[None, :]
    cst = np.concatenate([np.eye(128), (k <= t), np.ones((128, 128)), (k > t), (k < t)], axis=1).astype(f32)
    lnbc = np.stack([inp["ln_in_g"], inp["ln_in_b"], inp["ln1_g"][0], inp["ln1_b"][0], inp["ln2_g"][0],
                     inp["ln2_b"][0]]).astype(f32)
    lncol = np.concatenate([np.asarray(inp["ln_in_g"], f32).reshape(8, P).T,
                            np.asarray(inp["ln_in_b"], f32).reshape(8, P).T], axis=1)
    rb = np.asarray(inp["rel_bias"], f32)
    s_ = np.arange(128)[:, None]
    q_ = np.arange(128)[None, :]
    biasT = np.zeros((P, 2, 2, 2, 4, 128), f32)
    maskT = np.zeros((P, 2, 128), f32)
    for ch in range(2):
        dist = q_ + 128 - (s_ + ch * 128)
        valid = (dist >= 0) & (dist < 128)
        bucket = _t5_bucket(np.clip(dist, 0, None))
        maskT[:, ch, :] = np.where(valid, 0.0, -8.0e5)
        for kv in range(2):
            for par in range(2):
                for i in range(4):
                    biasT[:, ch, kv, par, i, :] = rb[bucket, kv * 8 + 2 * i + par]
    small = np.zeros((4, 32), f32)
    small[0, :16] = inp["attn_sink"][0]
    small[1] = inp["dt_bias"][0]
    small[2] = inp["a_log"][0]
    small[3] = inp["d_skip"][0]
    cw = np.asarray(inp["conv_w"][0], f32)
    convw = cw.reshape(4, 24, P).transpose(2, 1, 0).reshape(P, 96)
    convb = np.asarray(inp["conv_b"][0], f32).reshape(24, P).T
    bgate = np.asarray(inp["b_gate"][0], f32).reshape(16, P).T
    normg = np.asarray(inp["ssm_norm_g"][0], f32).reshape(1, 2048)
    wrr = np.concatenate([inp["w_group_router"][0], inp["w_expert_router"][0]], axis=1).astype(f32)
    wr = wrr.reshape(8, P, 36).transpose(1, 0, 2).reshape(P, 288)
    ebase = np.tile((np.arange(32) * CAP).astype(f32)[None, :], (P, 1))
    c = np.ascontiguousarray
    return dict(cst=c(cst), lnbc=c(lnbc), lncol=c(lncol), biasT=c(biasT.reshape(P, 4096)),
                maskT=c(maskT.reshape(P, 256)), small=c(small), convw=c(convw), convb=c(convb), bgate=c(bgate),
                normg=c(normg), wr=c(wr), ebase=c(ebase), wall=c(wall), wexp=c(wexp))


def kernel(**inputs):
    x = np.asarray(inputs["x"], np.float32)
    shared = _prep_shared(inputs)
    nc = build_program()
    in_maps = []
    for c in range(NCORE):
        m = dict(shared)
        m["x"] = np.ascontiguousarray(x[c * NSEQ:(c + 1) * NSEQ].reshape(NSEQ * SEQ, D))
        in_maps.append(m)
    res = run_bass_kernel_spmd(nc, in_maps, core_ids=list(range(NCORE)))
    outs = [np.asarray(r["out"], np.float32).reshape(NSEQ, SEQ, D) for r in res.results]
    return np.concatenate(outs, axis=0)
```
